# Optimizing a Trainium2 kernel written in Bass

```python
import jax, jax.numpy as jnp
from jax import lax
import numpy as np

D_MODEL = 1024
BATCH = 16
SEQ = 2048
DEPTH = 1

HEAD_DIM = 64
ATTN_PAIRS = ((128, 1), (512, 4), (2048, 16))
ATTN_GROUPS = 3
ATTN_HEADS_PER_GROUP = 8
ATTN_BLOCK = 64
ROPE_THETA = 10000.0
RET_HEADS = 8
RET_QK_DIM = 64
RET_V_DIM = 128
RET_CHUNK = 128
MOE_GROUPS = 8
MOE_EXPERTS_PER_GROUP = 8
MOE_N_EXPERTS = MOE_GROUPS * MOE_EXPERTS_PER_GROUP
MOE_TOP_K = 2
MOE_HIDDEN = 512
MOE_BLOCK = 128
NORM_EPS = 1e-6

ATTN_W = ATTN_GROUPS * ATTN_HEADS_PER_GROUP * HEAD_DIM
ATTN_OUT_W = ATTN_HEADS_PER_GROUP * HEAD_DIM
RET_QK_W = RET_HEADS * RET_QK_DIM
RET_V_W = RET_HEADS * RET_V_DIM
IN_SPLIT_POINTS = (ATTN_W, 2 * ATTN_W, 3 * ATTN_W,
                   3 * ATTN_W + RET_QK_W, 3 * ATTN_W + 2 * RET_QK_W,
                   3 * ATTN_W + 2 * RET_QK_W + RET_V_W,
                   3 * ATTN_W + 2 * RET_QK_W + 2 * RET_V_W)
IN_W = IN_SPLIT_POINTS[-1] + 2 * D_MODEL

kernel_name = 'gated_hybrid_dilated_attn_retention_hmoe'


def rms_norm(x, w):
    xf = x.astype(jnp.float32)
    y = xf * lax.rsqrt(jnp.mean(xf * xf, axis=-1, keepdims=True) + NORM_EPS)
    return (y * w.astype(jnp.float32)).astype(x.dtype)


def rotary_tables(seq_len):
    inv_freq = 1.0 / (ROPE_THETA ** (jnp.arange(0, HEAD_DIM, 2, dtype=jnp.float32) / HEAD_DIM))
    ang = jnp.arange(seq_len, dtype=jnp.float32)[:, None] * inv_freq[None, :]
    return jnp.cos(ang), jnp.sin(ang)


def apply_rotary(t, cos, sin):
    t1, t2 = jnp.split(t.astype(jnp.float32), 2, axis=-1)
    c = cos[None, :, None, :]
    s = sin[None, :, None, :]
    return jnp.concatenate([t1 * c - t2 * s, t1 * s + t2 * c], axis=-1)


def dilated_window_attention(q, k, v, dilation, half):
    B, S, H, Dh = q.shape
    L = S // dilation
    nb = -(-L // ATTN_BLOCK)
    Lp = nb * ATTN_BLOCK
    N = B * dilation

    def strided(t):
        t = t.reshape(B, L, dilation, H, Dh).transpose(0, 2, 3, 1, 4)
        return t.reshape(N, H, L, Dh)

    qb = jnp.pad(strided(q), ((0, 0), (0, 0), (0, Lp - L), (0, 0))).reshape(N, H, nb, ATTN_BLOCK, Dh)

    def band(t):
        tp = jnp.pad(strided(t), ((0, 0), (0, 0), (ATTN_BLOCK, Lp - L + ATTN_BLOCK), (0, 0)))
        tb = tp.reshape(N, H, nb + 2, ATTN_BLOCK, Dh)
        return jnp.concatenate([tb[:, :, :-2], tb[:, :, 1:-1], tb[:, :, 2:]], axis=3)

    kw = band(k)
    vw = band(v)
    scores = jnp.einsum('nhbqd,nhbkd->nhbqk', qb, kw) * (Dh ** -0.5)
    blk = jnp.arange(nb)[:, None] * ATTN_BLOCK
    q_pos = blk + jnp.arange(ATTN_BLOCK)[None, :]
    k_pos = blk - ATTN_BLOCK + jnp.arange(3 * ATTN_BLOCK)[None, :]
    rel = k_pos[:, None, :] - q_pos[:, :, None]
    in_range = (k_pos[:, None, :] >= 0) & (k_pos[:, None, :] < L)
    valid = (jnp.abs(rel) <= half) & (in_range | (rel == 0))
    scores = jnp.where(valid[None, None], scores, -jnp.inf)
    m = jnp.max(scores, axis=-1, keepdims=True)
    p = jnp.exp(scores - m)
    denom = jnp.sum(p, axis=-1, keepdims=True)
    out = jnp.einsum('nhbqk,nhbkd->nhbqd', p, vw) / denom
    lse = (m + jnp.log(denom))[..., 0]
    out = out.reshape(N, H, Lp, Dh)[:, :, :L].reshape(B, dilation, H, L, Dh)
    out = out.transpose(0, 3, 1, 2, 4).reshape(B, S, H, Dh)
    lse = lse.reshape(N, H, Lp)[:, :, :L].reshape(B, dilation, H, L)
    lse = lse.transpose(0, 3, 1, 2).reshape(B, S, H)
    return out, lse


def retention_chunkwise(q, k, v, log_g):
    B, H, S, Dk = q.shape
    Dv = v.shape[-1]
    C = RET_CHUNK
    nc = S // C
    qc = q.reshape(B, H, nc, C, Dk)
    kc = k.reshape(B, H, nc, C, Dk)
    vc = v.reshape(B, H, nc, C, Dv)
    idx = jnp.arange(C, dtype=jnp.float32)
    rel = idx[:, None] - idx[None, :]
    decay = jnp.where(rel[None] >= 0, jnp.exp(jnp.maximum(rel[None], 0.0) * log_g[:, None, None]), 0.0)
    scores = jnp.einsum('bhnqd,bhnkd->bhnqk', qc, kc) * decay[None, :, None]
    inner = jnp.einsum('bhnqk,bhnkv->bhnqv', scores, vc)
    zeta = jnp.exp((C - 1 - idx)[None, :] * log_g[:, None])
    kv = jnp.einsum('bhnkd,bhnkv->nbhdv', kc * zeta[None, :, None, :, None], vc)
    chunk_decay = jnp.exp(C * log_g)[None, :, None, None]

    def step(state, kv_chunk):
        return chunk_decay * state + kv_chunk, state

    _, prev_states = lax.scan(step, jnp.zeros((B, H, Dk, Dv), jnp.float32), kv)
    xi = jnp.exp((idx + 1.0)[None, :] * log_g[:, None])
    cross = jnp.einsum('bhnqd,nbhdv->bhnqv', qc * xi[None, :, None, :, None], prev_states)
    return (inner + cross).reshape(B, H, S, Dv)


def hybrid_mixer(xn, w_in, b_branch_gate, ret_decay_fwd, ret_decay_bwd, ret_gn_w,
                 w_attn_branch, w_ret_branch, w_out, cos, sin):
    B, S, _ = xn.shape
    proj = jnp.einsum('bsd,de->bse', xn, w_in)
    q_a, k_a, v_a, q_r, k_r, v_r, g_r, gate_logits = jnp.split(proj, IN_SPLIT_POINTS, axis=-1)

    n_a = ATTN_GROUPS * ATTN_HEADS_PER_GROUP
    q_a = apply_rotary(q_a.reshape(B, S, n_a, HEAD_DIM), cos, sin).reshape(B, S, ATTN_GROUPS, ATTN_HEADS_PER_GROUP, HEAD_DIM)
    k_a = apply_rotary(k_a.reshape(B, S, n_a, HEAD_DIM), cos, sin).reshape(B, S, ATTN_GROUPS, ATTN_HEADS_PER_GROUP, HEAD_DIM)
    v_a = v_a.astype(jnp.float32).reshape(B, S, ATTN_GROUPS, ATTN_HEADS_PER_GROUP, HEAD_DIM)
    outs = []
    lses = []
    for g, (window, dilation) in enumerate(ATTN_PAIRS):
        o, l = dilated_window_attention(q_a[:, :, g], k_a[:, :, g], v_a[:, :, g], dilation, window // (2 * dilation))
        outs.append(o)
        lses.append(l)
    mix_w = jax.nn.softmax(jnp.stack(lses, axis=0), axis=0)
    y_att = jnp.einsum('gbsh,gbshd->bshd', mix_w, jnp.stack(outs, axis=0)).reshape(B, S, ATTN_OUT_W)

    q_r = apply_rotary(q_r.reshape(B, S, RET_HEADS, RET_QK_DIM), cos, sin).transpose(0, 2, 1, 3)
    k_r = (apply_rotary(k_r.reshape(B, S, RET_HEADS, RET_QK_DIM), cos, sin) * (RET_QK_DIM ** -0.5)).transpose(0, 2, 1, 3)
    v_r = v_r.astype(jnp.float32).reshape(B, S, RET_HEADS, RET_V_DIM).transpose(0, 2, 1, 3)
    log_g_fwd = jax.nn.log_sigmoid(ret_decay_fwd.astype(jnp.float32))
    log_g_bwd = jax.nn.log_sigmoid(ret_decay_bwd.astype(jnp.float32))
    ret_f = retention_chunkwise(q_r, k_r, v_r, log_g_fwd)
    ret_b = jnp.flip(retention_chunkwise(jnp.flip(q_r, 2), jnp.flip(k_r, 2), jnp.flip(v_r, 2), log_g_bwd), 2)
    ret = ret_f + ret_b
    mu = jnp.mean(ret, axis=-1, keepdims=True)
    var = jnp.mean(jnp.square(ret - mu), axis=-1, keepdims=True)
    ret = (ret - mu) * lax.rsqrt(var + NORM_EPS)
    ret = ret.transpose(0, 2, 1, 3).reshape(B, S, RET_V_W) * ret_gn_w.astype(jnp.float32)
    y_ret = jax.nn.silu(g_r.astype(jnp.float32)) * ret

    gates = jax.nn.sigmoid((gate_logits + b_branch_gate).astype(jnp.float32))
    g_att, g_ret = jnp.split(gates, 2, axis=-1)
    merged = (g_att * jnp.einsum('bse,ed->bsd', y_att, w_attn_branch.astype(jnp.float32))
              + g_ret * jnp.einsum('bse,ed->bsd', y_ret, w_ret_branch.astype(jnp.float32)))
    return jnp.einsum('bsd,de->bse', merged.astype(xn.dtype), w_out)


def hierarchical_moe(x2, w_group, b_group, w_expert, b_expert, w1, w3, w2):
    T, D = x2.shape
    xf = x2.astype(jnp.float32)
    group_p = jax.nn.softmax(xf @ w_group.astype(jnp.float32) + b_group.astype(jnp.float32), axis=-1)
    g_w, g_idx = lax.top_k(group_p, 1)
    exp_logits = (xf @ w_expert.astype(jnp.float32) + b_expert.astype(jnp.float32)).reshape(T, MOE_GROUPS, MOE_EXPERTS_PER_GROUP)
    sel_logits = jnp.take_along_axis(exp_logits, g_idx[:, :, None], axis=1)[:, 0]
    e_w, e_idx = lax.top_k(jax.nn.softmax(sel_logits, axis=-1), MOE_TOP_K)
    e_w = e_w / jnp.sum(e_w, axis=-1, keepdims=True)
    gate = (g_w * e_w).reshape(-1)
    eid = (g_idx * MOE_EXPERTS_PER_GROUP + e_idx).reshape(-1).astype(jnp.int32)
    tok = jnp.repeat(jnp.arange(T, dtype=jnp.int32), MOE_TOP_K)
    A = T * MOE_TOP_K

    order = jnp.argsort(eid)
    s_eid = eid[order]
    s_tok = tok[order]
    s_gate = gate[order]
    counts = jnp.bincount(eid, length=MOE_N_EXPERTS)
    starts = jnp.cumsum(counts) - counts
    padded = (counts + MOE_BLOCK - 1) // MOE_BLOCK * MOE_BLOCK
    p_ends = jnp.cumsum(padded)
    p_starts = p_ends - padded
    dest = p_starts[s_eid] + (jnp.arange(A, dtype=jnp.int32) - starts[s_eid])
    n_blocks = -(-A // MOE_BLOCK) + MOE_N_EXPERTS
    slot_tok = jnp.full((n_blocks * MOE_BLOCK,), T, jnp.int32).at[dest].set(s_tok)
    blk_eid = jnp.clip(jnp.searchsorted(p_ends, jnp.arange(n_blocks) * MOE_BLOCK, side='right'), 0, MOE_N_EXPERTS - 1)
    x_pad = jnp.concatenate([x2, jnp.zeros((1, D), x2.dtype)], axis=0)
    x_slots = x_pad[slot_tok].reshape(n_blocks, MOE_BLOCK, D)

    def expert_block(args):
        xb, e = args
        hid = jax.nn.silu(xb @ w1[e]) * (xb @ w3[e])
        return hid @ w2[e]

    y_slots = lax.map(expert_block, (x_slots, blk_eid)).reshape(n_blocks * MOE_BLOCK, D)
    y_assign = y_slots[dest].astype(jnp.float32) * s_gate[:, None]
    return jax.ops.segment_sum(y_assign, s_tok, num_segments=T).astype(x2.dtype)


def setup_inputs(seed: int = 0) -> dict:
    key = jax.random.key(seed)
    ks = jax.random.split(key, 20)
    L = DEPTH
    f32 = jnp.float32

    def nrm(k, shape, scale):
        return jax.random.normal(k, shape, f32) * scale

    heads = jnp.arange(RET_HEADS, dtype=f32)
    decay_logit = jnp.log(2.0 ** (5.0 + heads) - 1.0)
    return {
        'x': nrm(ks[0], (BATCH, SEQ, D_MODEL), 1.0),
        'norm_mix_w': 1.0 + nrm(ks[1], (L, D_MODEL), 0.02),
        'w_in': nrm(ks[2], (L, D_MODEL, IN_W), D_MODEL ** -0.5),
        'b_branch_gate': nrm(ks[3], (L, 2 * D_MODEL), 0.02),
        'ret_decay_fwd': decay_logit[None] + nrm(ks[4], (L, RET_HEADS), 0.05),
        'ret_decay_bwd': decay_logit[None] + nrm(ks[5], (L, RET_HEADS), 0.05),
        'ret_gn_w': 1.0 + nrm(ks[6], (L, RET_V_W), 0.02),
        'w_attn_branch': nrm(ks[7], (L, ATTN_OUT_W, D_MODEL), ATTN_OUT_W ** -0.5),
        'w_ret_branch': nrm(ks[8], (L, RET_V_W, D_MODEL), RET_V_W ** -0.5),
        'w_out': nrm(ks[9], (L, D_MODEL, D_MODEL), D_MODEL ** -0.5),
        'norm_moe_w': 1.0 + nrm(ks[10], (L, D_MODEL), 0.02),
        'moe_w_group': nrm(ks[11], (L, D_MODEL, MOE_GROUPS), D_MODEL ** -0.5),
        'moe_b_group': nrm(ks[12], (L, MOE_GROUPS), 0.01),
        'moe_w_expert': nrm(ks[13], (L, D_MODEL, MOE_N_EXPERTS), D_MODEL ** -0.5),
        'moe_b_expert': nrm(ks[14], (L, MOE_N_EXPERTS), 0.01),
        'moe_w1': nrm(ks[15], (L, MOE_N_EXPERTS, D_MODEL, MOE_HIDDEN), D_MODEL ** -0.5),
        'moe_w3': nrm(ks[16], (L, MOE_N_EXPERTS, D_MODEL, MOE_HIDDEN), D_MODEL ** -0.5),
        'moe_w2': nrm(ks[17], (L, MOE_N_EXPERTS, MOE_HIDDEN, D_MODEL), MOE_HIDDEN ** -0.5),
        'norm_final_w': 1.0 + nrm(ks[18], (D_MODEL,), 0.02),
    }


def reference(x, norm_mix_w, w_in, b_branch_gate, ret_decay_fwd, ret_decay_bwd, ret_gn_w,
              w_attn_branch, w_ret_branch, w_out, norm_moe_w, moe_w_group, moe_b_group,
              moe_w_expert, moe_b_expert, moe_w1, moe_w3, moe_w2, norm_final_w):
    B, S, D = x.shape
    cos, sin = rotary_tables(S)
    h = x
    for l in range(DEPTH):
        xn = rms_norm(h, norm_mix_w[l])
        mix = hybrid_mixer(xn, w_in[l], b_branch_gate[l], ret_decay_fwd[l], ret_decay_bwd[l], ret_gn_w[l],
                           w_attn_branch[l], w_ret_branch[l], w_out[l], cos, sin)
        h = h + mix.astype(h.dtype)
        hn = rms_norm(h, norm_moe_w[l])
        ffn = hierarchical_moe(hn.reshape(B * S, D), moe_w_group[l], moe_b_group[l], moe_w_expert[l],
                               moe_b_expert[l], moe_w1[l], moe_w3[l], moe_w2[l])
        h = h + ffn.reshape(B, S, D).astype(h.dtype)
    return rms_norm(h, norm_final_w)
```

```python
import contextlib
import numpy as np
import concourse.bass as bass
import concourse.mybir as mybir
from concourse.bass_utils import run_bass_kernel_spmd

F32 = mybir.dt.float32
BF16 = mybir.dt.bfloat16
AF = mybir.ActivationFunctionType
ALU = mybir.AluOpType
AX = mybir.AxisListType

S = 2048
D = 1024
NSEQ = 2
EPS = 1e-6
N_EXP = 64


class Prog:
    ENGS = ("pe", "act", "dve", "pool", "sp")

    def __init__(self, nc):
        self.nc = nc
        self.stream = {e: [] for e in self.ENGS}
        self.cnt = {e: 0 for e in self.ENGS}
        self.pending = {e: False for e in self.ENGS}
        self.waited = {}
        self.lastw = {}
        self.readers = {}
        self.dma_cnt = {}
        self.sems = {}
        self.nops = 0
        self.maxops = None
        self.force = False

    def _cut(self):
        if self.force:
            return False
        self.nops += 1
        return self.maxops is not None and self.nops > self.maxops

    def _deps(self, eng, reads, writes, skip_same=False):
        need = {}

        def add(rec):
            s, v = rec
            if s == "pe" and eng == "pe":
                return
            if skip_same and s == eng:
                return
            if need.get(s, 0) < v:
                need[s] = v

        for k in reads:
            if k in self.lastw:
                add(self.lastw[k])
        for k in writes:
            if k in self.lastw:
                add(self.lastw[k])
            for s, v in self.readers.get(k, {}).items():
                add((s, v))
        waits = []
        for s, v in need.items():
            if self.waited.get((eng, s), 0) < v:
                self.waited[(eng, s)] = v
                waits.append((s, v))
        return waits

    def _commit(self, rec, reads, writes):
        s, v = rec
        for k in reads:
            d = self.readers.setdefault(k, {})
            if d.get(s, 0) < v:
                d[s] = v
        for k in writes:
            self.lastw[k] = rec
            self.readers[k] = {}

    def op(self, eng, fn, reads=(), writes=(), inc=True, skip_same=False):
        if self._cut():
            return
        rd, wr = [], list(writes)
        for k in reads:
            (wr if k.startswith("ps") else rd).append(k)
        reads, writes = rd, wr
        waits = self._deps(eng, reads, writes, skip_same)
        rec = (eng, self.cnt[eng] + 1)
        if inc:
            self.cnt[eng] += 1
            self.pending[eng] = False
            self.stream[eng].append((waits, fn, (eng, 1)))
        else:
            self.pending[eng] = True
            self.stream[eng].append((waits, fn, None))
        self._commit(rec, reads, writes)

    def dma(self, q, slot, fn, reads=(), writes=()):
        if self._cut():
            return
        s = "dma:" + str(slot)
        waits = self._deps(q, reads, writes)
        self.dma_cnt[s] = self.dma_cnt.get(s, 0) + 16
        rec = (s, self.dma_cnt[s])
        self.stream[q].append((waits, fn, (s, 16)))
        self._commit(rec, reads, writes)

    def barrier(self):
        for e in self.ENGS:
            assert not self.pending[e]
        tot = {e: self.cnt[e] for e in self.ENGS}
        tot.update(self.dma_cnt)
        for e in self.ENGS:
            waits = []
            for s, v in tot.items():
                if s == e or v == 0:
                    continue
                if self.waited.get((e, s), 0) < v:
                    self.waited[(e, s)] = v
                    waits.append((s, v))
            self.stream[e].append((waits, None, None))

    def wait_all(self, eng, keys):
        waits = self._deps(eng, keys, ())
        self.stream[eng].append((waits, None, None))

    def finish(self):
        nc = self.nc
        names = list(self.ENGS) + sorted(self.dma_cnt.keys())
        with contextlib.ExitStack() as st:
            for n in names:
                self.sems[n] = st.enter_context(nc.semaphore(n.replace(":", "_")))
            block = st.enter_context(nc.Block())
            sems = self.sems

            def run(engname, e):
                for waits, fn, inc in self.stream[engname]:
                    for s, v in waits:
                        e.wait_ge(sems[s], v)
                    if fn is not None:
                        ins = fn(e)
                        if inc is not None:
                            ins.then_inc(sems[inc[0]], inc[1])

            @block.tensor
            def _(e):
                run("pe", e)

            @block.scalar
            def _(e):
                run("act", e)

            @block.vector
            def _(e):
                run("dve", e)

            @block.gpsimd
            def _(e):
                run("pool", e)

            @block.sync
            def _(e):
                run("sp", e)


BLK_QA, BLK_KA, BLK_VA = 0, 12, 24
BLK_QR, BLK_KR, BLK_VR, BLK_GR, BLK_GATE = 36, 40, 44, 52, 60
ATTN_DIL = (1, 4, 16)


def build(stage="full", nseq=NSEQ):
    nc = bass.Bass("TRN2", target_bir_lowering=False)
    T = nseq * S

    def din(name, shape, dt=F32):
        return nc.dram_tensor(name, list(shape), dt, kind="ExternalInput").ap()

    x_d = din("x", [NSEQ * S, D])
    win_d = din("win", [76, 128, 8, 128])
    wa_d = din("wa", [8, 128, 4, 128])
    wb_d = din("wb", [8, 128, 8, 128])
    wo_d = din("wo", [128, 8, 1024])
    nw_d = din("nw", [3, 128, 1024])
    cs_d = din("cs", [2, 128, S])
    sm_d = din("sm", [128, 64])
    c128_d = din("c128", [6, 128, 128])
    bm_d = din("bm", [128, 256])
    iod_d = din("iod", [128, 16])
    wr_d = din("wr", [128, 8, 72])
    br_d = din("br", [128, 72])
    pidx_d = din("pidx", [64, 128])
    if stage == "full":
        w1_d = din("w1", [N_EXP, 1024, 512])
        w3_d = din("w3", [N_EXP, 1024, 512])
        w2_d = din("w2", [N_EXP, 512, 1024])
    out_d = nc.dram_tensor("out", [NSEQ * S, D], F32, kind="ExternalOutput").ap()
    h_d = nc.dram_tensor("hscr", [NSEQ * S, D], F32, kind="Internal").ap()

    st = contextlib.ExitStack()
    with st:
        def sb(name, shape, dt):
            return st.enter_context(nc.sbuf_tensor("s_" + name, list(shape), dt))

        xnT_t = sb("xnT", [128, 8 * S], BF16)
        xnT = xnT_t[:].rearrange("p (c t) -> p c t", c=8)
        cos_t = sb("cos", [128, S], F32)
        sin_t = sb("sin", [128, S], F32)
        ar1 = sb("ar1", [128, 32768], BF16)
        ar2 = sb("ar2", [128, 24576], BF16)
        vaug = [sb(f"vaug{i}", [128, 2048], BF16) for i in range(2)]
        Fs = [sb(f"F{i}", [128, 512], F32) for i in range(7)]
        Ps = [sb(f"P{i}", [128, 512], BF16) for i in range(4)]
        Ws = [sb(f"W{i}", [128, 1024], BF16) for i in range(6)]
        sm = sb("sm", [128, 64], F32)
        c128 = sb("c128", [128, 6 * 128], F32)
        identb = sb("identb", [128, 128], BF16)
        bm = sb("bm", [128, 256], BF16)
        iod = sb("iod", [128, 16], F32)
        lg = sb("lg", [128, 16], F32)
        sc = sb("sc", [128, 32], F32)
        ef = sb("ef", [128, 256], F32)
        small = sb("small", [128, 64], F32)
        onesf = sb("onesf", [128, 128], F32)
        wr = sb("wrs", [128, 8 * 72], F32)
        br = sb("brs", [128, 72], F32)
        pidx = sb("pidx", [64, 128], F32)
        sel = sb("sel", [64, 128], F32)
        ps = [st.enter_context(nc.psum_tensor(f"ps{i}", [128, 512], F32)) for i in range(8)]

        p = Prog(nc)
        import os
        if os.environ.get("KCUT"):
            p.maxops = int(os.environ["KCUT"])

        y_attT = ar1[:, 0:8192].rearrange("p (c t) -> p c t", c=4)
        y_retT = ar1[:, 8192:24576].rearrange("p (c t) -> p c t", c=8)
        acc = [ar1[:, 24576 + i * 4096: 24576 + (i + 1) * 4096].bitcast(F32) for i in range(2)]
        y_acc = ar1[:].bitcast(F32).rearrange("p (t d) -> p t d", d=1024)
        G = [ar2[:, i * 2048:(i + 1) * 2048] for i in range(8)]
        MASK = ar2[:, 16384:24576].bitcast(F32)
        ident = c128[:, 0:128]
        relp = c128[:, 128:256]
        reln = c128[:, 256:384]
        r128f = c128[:, 384:512]
        r128b = c128[:, 512:640]
        eye8 = c128[:, 640:768]
        wrv = wr[:].rearrange("p (c f) -> p c f", c=8)
        gateT = cos_t[0:64, :]

        rr = {}
        outkeys = []

        def rot(name, n):
            i = rr.get(name, 0)
            rr[name] = i + 1
            return i % n

        def act(out, in_, func, reads, writes, **kw):
            p.op("act", lambda e: e.activation(out=out, in_=in_, func=func, **kw), reads, writes)

        def tt(eng, out, in0, in1, op, reads, writes, **kw):
            p.op(eng, lambda e: e.tensor_tensor(out=out, in0=in0, in1=in1, op=op), reads, writes, **kw)

        def ts(eng, out, in0, s1, s2, op0, op1, reads, writes, **kw):
            if op1 is None:
                p.op(eng, lambda e: e.tensor_scalar(out=out, in0=in0, scalar1=s1, scalar2=None, op0=op0), reads, writes, **kw)
            else:
                p.op(eng, lambda e: e.tensor_scalar(out=out, in0=in0, scalar1=s1, scalar2=s2, op0=op0, op1=op1), reads, writes, **kw)

        def stt(out, in0, scalar, in1, op0, op1, reads, writes):
            p.op("dve", lambda e: e.scalar_tensor_tensor(out=out, in0=in0, scalar=scalar, in1=in1, op0=op0, op1=op1), reads, writes)

        def mm(bank, out, lhsT, rhs, start, stop, reads, last=None, **kw):
            if last is None:
                last = stop
            p.op("pe", lambda e: e.matmul(out, lhsT, rhs, start=start, stop=stop, **kw), reads, [f"ps{bank}"], inc=last)

        def load(q, slot, out, in_, writes, reads=()):
            p.dma(q, slot, lambda e: e.dma_start(out=out, in_=in_), reads, writes)

        load("sp", "sm", sm[:], sm_d, ["sm"])
        load("sp", "c128", c128[:].rearrange("p (k f) -> p k f", k=6), c128_d.rearrange("k p f -> p k f"), ["c128"])
        load("pool", "identb", identb[:], c128_d[0], ["identb"])
        load("pool", "bm", bm[:], bm_d, ["bm"])
        load("sp", "iod", iod[:], iod_d, ["iod"])
        load("sp", "wr", wrv, wr_d, ["wr"])
        load("sp", "br", br[:], br_d, ["br"])
        load("sp", "pidx", pidx[:], pidx_d, ["pidx"])
        p.op("pool", lambda e: e.memset(onesf[:], 1.0 / 128.0), (), ["onesf"])
        p.op("pool", lambda e: e.memset(small[:, 0:1], EPS), (), ["small0"])
        p.op("pool", lambda e: e.memset(small[:, 1:2], float(np.log(0.125))), (), ["small1"])
        act(lg[:], sm[:, 24:40], AF.Exp, ["sm"], ["lg"], scale=-1.0)
        act(lg[:], lg[:], AF.Ln, ["lg"], ["lg"], bias=1.0)
        ts("dve", lg[:], lg[:], -1.0, None, ALU.mult, None, ["lg"], ["lg"])
        eps_ap = small[:, 0:1]
        ln8_ap = small[:, 1:2]

        def rstd_from_ss(ss, scale, rkey):
            act(ss, ss, AF.Sqrt, [rkey, "small0"], [rkey], scale=scale, bias=eps_ap)
            p.op("dve", lambda e: e.reciprocal(out=ss, in_=ss), [rkey], [rkey])

        for s in range(nseq):
            tok0 = s * S
            load("sp", "cos", cos_t[:], cs_d[0], ["cos"])
            load("sp", "sin", sin_t[:], cs_d[1], ["sin"])
            nwk = "G7"
            load("sp", "G7", G[7].bitcast(F32), nw_d[0], [nwk])
            for t in range(16):
                gi = rot("xg", 2)
                xt = G[gi].bitcast(F32)
                load("sp", f"G{gi}", xt, x_d[tok0 + t * 128: tok0 + (t + 1) * 128, :], [f"G{gi}"])
                fi = rot("F", 7)
                ss = small[:, 8 + (t % 4): 9 + (t % 4)]
                sk = f"ss{t % 4}"
                act(Fs[fi][:].bitcast(BF16), xt, AF.Square, [f"G{gi}"], [f"F{fi}", sk], accum_out=ss)
                rstd_from_ss(ss, 1.0 / D, sk)
                xb = G[2 + rot("xb", 2)]
                xbk = f"G{2 + (rr['xb'] - 1) % 2}"
                stt(xb[:, 0:1024], xt, ss, G[7].bitcast(F32), ALU.mult, ALU.mult, [f"G{gi}", sk, nwk], [xbk])
                for half in range(2):
                    b = rot("pst", 2)
                    pv = ps[b][:].bitcast(BF16)
                    for j in range(4):
                        kc = half * 4 + j
                        p.op("pe", lambda e, pv=pv, j=j, kc=kc, xb=xb: e.transpose(out=pv[:, j * 128:(j + 1) * 128], in_=xb[:, kc * 128:(kc + 1) * 128], identity=identb[:]),
                             [xbk, "identb"], [f"ps{b}"], inc=(j == 3))
                    o = xnT[:, half * 4:half * 4 + 4, t * 128:(t + 1) * 128]
                    i_ = pv[:, 0:512].rearrange("p (c t) -> p c t", c=4)
                    if half == 0:
                        act(o, i_, AF.Copy, [f"ps{b}"], [f"xnT{t // 4}"])
                    else:
                        p.op("dve", lambda e, o=o, i_=i_: e.tensor_copy(out=o, in_=i_), [f"ps{b}"], [f"xnT{t // 4}"])

            if stage == "s0":
                for c in range(8):
                    p.dma("pool", "dbg", lambda e, c=c: e.dma_start(out=out_d[c * 256:(c + 1) * 256, :].rearrange("(p two) f -> p (two f)", two=2), in_=xnT[:, c, :]),
                          [f"xnT{q}" for q in range(4)], [f"dbg{c}"])
                    outkeys.append(f"dbg{c}")
                continue
            def load_w(blk):
                wi = rot("W", 6)
                load("pool", f"W{wi}", Ws[wi][:].rearrange("p (c f) -> p c f", c=8), win_d[blk], [f"W{wi}"])
                return wi

            def proj_fm(wi, n, bank):
                wv = Ws[wi][:].rearrange("p (c f) -> p c f", c=8)
                for kc in range(8):
                    mm(bank, ps[bank][:], wv[:, kc, :], xnT[:, kc, n * 512:(n + 1) * 512], kc == 0, kc == 7,
                       [f"W{wi}", f"xnT{n}"])

            def rotary(bank, n, dst, dstk, dil):
                fa, fb = rot("F", 7), rot("F", 7)
                A, B = Fs[fa], Fs[fb]
                tsl = slice(n * 512, (n + 1) * 512)
                tt("dve", A[:], ps[bank][:], cos_t[:, tsl], ALU.mult, [f"ps{bank}", "cos"], [f"F{fa}"])
                for q4 in range(4):
                    src = (q4 ^ 1) * 32
                    tt("dve", B[q4 * 32:(q4 + 1) * 32, :], ps[bank][src:src + 32, :], sin_t[src:src + 32, tsl], ALU.mult,
                       [f"ps{bank}", "sin"], [f"F{fb}"], skip_same=(q4 > 0))
                L = S // dil
                if dil == 1:
                    o = dst[:, tsl]
                    ia, ib = A[:], B[:]
                else:
                    o = dst.rearrange("p (r m) -> p r m", r=dil)[:, :, n * 512 // dil:(n + 1) * 512 // dil]
                    ia = A[:].rearrange("p (m r) -> p r m", r=dil)
                    ib = B[:].rearrange("p (m r) -> p r m", r=dil)
                tt("pool", o, ia, ib, ALU.add, [f"F{fa}", f"F{fb}"], [dstk])

            for hp in range(4):
                wq = load_w(BLK_QR + hp)
                wk = load_w(BLK_KR + hp)
                gq, gk = rot("Gqk", 4), rot("Gqk", 4)
                qT, kT = G[gq], G[gk]
                for n in range(4):
                    b = rot("psp", 2)
                    proj_fm(wq, n, b)
                    rotary(b, n, qT, f"G{gq}", 1)
                    b = rot("psp", 2)
                    proj_fm(wk, n, b)
                    rotary(b, n, kT, f"G{gk}", 1)
                for hh in range(2):
                    H = 2 * hp + hh
                    hb = hh * 64
                    wv_i = load_w(BLK_VR + H)
                    wg_i = load_w(BLK_GR + H)
                    gv, gg = 4 + rot("Gv", 2), 6 + rot("Gg", 2)
                    vS = G[gv].rearrange("p (t f) -> p t f", t=16)
                    gS = G[gg]
                    wvv = Ws[wv_i][:].rearrange("p (c f) -> p c f", c=8)
                    for t4 in range(4):
                        b = rot("psp", 2)
                        for tl in range(4):
                            t = t4 * 4 + tl
                            for kc in range(8):
                                mm(b, ps[b][:, tl * 128:(tl + 1) * 128], xnT[:, kc, t * 128:(t + 1) * 128], wvv[:, kc, :],
                                   kc == 0, kc == 7, [f"W{wv_i}", f"xnT{t4}"], last=(kc == 7 and tl == 3))
                        act(vS[:, t4 * 4:(t4 + 1) * 4, :], ps[b][:].rearrange("p (t f) -> p t f", t=4), AF.Copy, [f"ps{b}"], [f"G{gv}"])
                    for n in range(4):
                        b = rot("psp", 2)
                        proj_fm(wg_i, n, b)
                        act(gS[:, n * 512:(n + 1) * 512], ps[b][:], AF.Silu, [f"ps{b}"], [f"G{gg}"])
                    lgf, lgb = lg[:, H:H + 1], lg[:, 8 + H:9 + H]
                    act(ef[:, 0:128], r128f, AF.Exp, ["c128", "lg", "small1"], ["ef0"], scale=lgf, bias=ln8_ap)
                    act(ef[:, 128:256], r128b, AF.Exp, ["c128", "lg", "small1"], ["ef1"], scale=lgb, bias=ln8_ap)
                    act(sc[:, 0:15], iod[:, 0:15], AF.Exp, ["iod", "lg"], ["sc0"], scale=lgf)
                    act(sc[:, 16:31], iod[:, 0:15], AF.Exp, ["iod", "lg"], ["sc1"], scale=lgb)
                    for dl in range(1, 16):
                        ts("pool", MASK[:, (15 + dl) * 128:(16 + dl) * 128], ef[:, 0:128], sc[:, dl - 1:dl], None, ALU.mult, None,
                           ["ef0", "sc0"], ["MASK"], skip_same=(dl > 1))
                        ts("pool", MASK[:, (15 - dl) * 128:(16 - dl) * 128], ef[:, 128:256], sc[:, 15 + dl:16 + dl], None, ALU.mult, None,
                           ["ef1", "sc1"], ["MASK"], skip_same=True)
                    fi = rot("F", 7)
                    ts("dve", Fs[fi][:, 0:128], relp, lgf, None, ALU.mult, None, ["c128", "lg"], [f"F{fi}"])
                    stt(Fs[fi][:, 128:256], reln, lgb, Fs[fi][:, 0:128], ALU.mult, ALU.add, ["c128", "lg", f"F{fi}"], [f"F{fi}"])
                    act(Fs[fi][:, 256:384], Fs[fi][:, 128:256], AF.Exp, [f"F{fi}", "small1"], [f"F{fi}"], bias=ln8_ap)
                    tt("pool", MASK[:, 15 * 128:16 * 128], Fs[fi][:, 256:384], eye8, ALU.add, [f"F{fi}", "c128"], ["MASK"])
                    items = [(n, cj) for n in range(4) for cj in range(16)]

                    def qk(i):
                        n, cj = items[i]
                        b = 2 + (i % 3)
                        mm(b, ps[b][:], kT[hb:hb + 64, cj * 128:(cj + 1) * 128], qT[hb:hb + 64, n * 512:(n + 1) * 512], True, True,
                           [f"G{gk}", f"G{gq}"])

                    qk(0)
                    qk(1)
                    for i, (n, cj) in enumerate(items):
                        b = 2 + (i % 3)
                        pi = i % 4
                        d0 = 4 * n - cj + 15
                        tt("dve", Ps[pi][:], ps[b][:], MASK[:, d0 * 128:(d0 + 4) * 128], ALU.mult, [f"ps{b}", "MASK"], [f"P{pi}"])
                        mm(5, ps[5][:], vS[:, cj, :], Ps[pi][:], cj == 0, cj == 15, [f"G{gv}", f"P{pi}"])
                        if i + 2 < len(items):
                            qk(i + 2)
                        if cj == 15:
                            f_oc, f_sq, f_mean, f_var = rot("F", 7), rot("F", 7), rot("F", 7), rot("F", 7)
                            Oc, Osq, Mn, Vr = Fs[f_oc], Fs[f_sq], Fs[f_mean], Fs[f_var]
                            act(Oc[:], ps[5][:], AF.Copy, ["ps5"], [f"F{f_oc}"])
                            act(Osq[:], ps[5][:], AF.Square, ["ps5"], [f"F{f_sq}"])
                            mm(6, ps[6][:], onesf[:], Oc[:], True, True, ["onesf", f"F{f_oc}"])
                            mm(7, ps[7][:], onesf[:], Osq[:], True, True, ["onesf", f"F{f_sq}"])
                            act(Mn[:], ps[6][:], AF.Copy, ["ps6"], [f"F{f_mean}"])
                            tt("pool", Osq[:], Mn[:], Mn[:], ALU.mult, [f"F{f_mean}"], [f"F{f_sq}"])
                            tt("dve", Vr[:], ps[7][:], Osq[:], ALU.subtract, ["ps7", f"F{f_sq}"], [f"F{f_var}"])
                            act(Vr[:], Vr[:], AF.Ln, [f"F{f_var}", "small0"], [f"F{f_var}"], bias=eps_ap)
                            act(Vr[:], Vr[:], AF.Exp, [f"F{f_var}"], [f"F{f_var}"], scale=-0.5)
                            tt("pool", Oc[:], Oc[:], Mn[:], ALU.subtract, [f"F{f_oc}", f"F{f_mean}"], [f"F{f_oc}"])
                            tt("pool", Oc[:], Oc[:], Vr[:], ALU.mult, [f"F{f_oc}", f"F{f_var}"], [f"F{f_oc}"])
                            stt(y_retT[:, H, n * 512:(n + 1) * 512], Oc[:], sm[:, 16 + H:17 + H], gS[:, n * 512:(n + 1) * 512],
                                ALU.mult, ALU.mult, [f"F{f_oc}", "sm", f"G{gg}"], [f"yret{n}"])

            if stage == "R":
                p.force = True
                for c in range(8):
                    p.dma("pool", "dbg", lambda e, c=c: e.dma_start(out=out_d[c * 256:(c + 1) * 256, :].rearrange("(p two) f -> p (two f)", two=2), in_=y_retT[:, c, :]),
                          [f"yret{q}" for q in range(4)], [f"dbg{c}"])
                    outkeys.append(f"dbg{c}")
                continue
            for i in range(2):
                p.op("pool", lambda e, i=i: e.memset(vaug[i][:], 1.0), (), [f"vaug{i}"])
            for hp in range(4):
                for g, dil in enumerate(ATTN_DIL):
                    L = S // dil
                    wq = load_w(BLK_QA + g * 4 + hp)
                    wk = load_w(BLK_KA + g * 4 + hp)
                    wv_i = load_w(BLK_VA + g * 4 + hp)
                    gq, gk = rot("Gqk", 4), rot("Gqk", 4)
                    qT, kT = G[gq], G[gk]
                    for n in range(4):
                        b = rot("psp", 2)
                        proj_fm(wq, n, b)
                        rotary(b, n, qT, f"G{gq}", dil)
                        b = rot("psp", 2)
                        proj_fm(wk, n, b)
                        rotary(b, n, kT, f"G{gk}", dil)
                    wvv = Ws[wv_i][:].rearrange("p (c f) -> p c f", c=8)
                    nj = L // 128
                    for t4 in range(4):
                        b = rot("psp", 2)
                        for tl in range(4):
                            ti = t4 * 4 + tl
                            r, j = ti // nj, ti % nj
                            base = 128 * j * dil + r
                            for kc in range(8):
                                lhs = xnT[:, kc, base: base + 127 * dil + 1: dil]
                                mm(b, ps[b][:, tl * 128:(tl + 1) * 128], lhs, wvv[:, kc, :], kc == 0, kc == 7,
                                   [f"W{wv_i}"] + [f"xnT{q}" for q in range(4)], last=(kc == 7 and tl == 3))
                        for hh in range(2):
                            va = vaug[hh][:].rearrange("p (t f) -> p t f", t=16)
                            src = ps[b][:].rearrange("p (t f) -> p t f", t=4)[:, :, hh * 64:(hh + 1) * 64]
                            if hh == 0:
                                act(va[:, t4 * 4:(t4 + 1) * 4, 0:64], src, AF.Copy, [f"ps{b}"], [f"vaug{hh}"])
                            else:
                                p.op("dve", lambda e, va=va, src=src, t4=t4: e.tensor_copy(out=va[:, t4 * 4:(t4 + 1) * 4, 0:64], in_=src),
                                     [f"ps{b}"], [f"vaug{hh}"])
                    for hh in range(2):
                        hb = hh * 64
                        va = vaug[hh][:].rearrange("p (t f) -> p t f", t=16)
                        pairs = []
                        for c in range(4):
                            first = True
                            u0 = 512 * c
                            segs = []
                            if L >= 512:
                                segs.append((u0 // L, u0 % L, u0 % L + 512))
                            else:
                                for r in range(u0 // L, (u0 + 512) // L):
                                    segs.append((r, 0, L))
                            for (r, m0, m1) in segs:
                                for j in range(nj):
                                    qa, qb = max(128 * j - 64, m0), min(128 * j + 192, m1)
                                    if qa < qb:
                                        pairs.append((c, r, j, qa, qb, first))
                                        first = False
                        lastidx = {}
                        for i, pr in enumerate(pairs):
                            lastidx[pr[0]] = i

                        def qk(i):
                            c, r, j, qa, qb, first = pairs[i]
                            b = 2 + (i % 3)
                            nq = qb - qa
                            mm(b, ps[b][:, 0:nq], kT[hb:hb + 64, r * L + 128 * j: r * L + 128 * j + 128], qT[hb:hb + 64, r * L + qa: r * L + qb],
                               True, True, [f"G{gk}", f"G{gq}"])

                        qk(0)
                        qk(1)
                        for i, (c, r, j, qa, qb, first) in enumerate(pairs):
                            b = 2 + (i % 3)
                            pi = i % 4
                            nq = qb - qa
                            off = qa - (128 * j - 64)
                            ub = r * L + qa - 512 * c
                            ob = 5 + (c % 2)
                            act(Ps[pi][:, 0:nq], ps[b][:, 0:nq], AF.Exp, [f"ps{b}"], [f"P{pi}"], scale=0.125)
                            tt("pool", Ps[pi][:, 0:nq], Ps[pi][:, 0:nq], bm[:, off:off + nq], ALU.mult, [f"P{pi}", "bm"], [f"P{pi}"])
                            islast = (lastidx[c] == i)
                            mm(ob, ps[ob][:, ub:ub + nq], va[:, r * nj + j, :], Ps[pi][:, 0:nq], first, islast,
                               [f"vaug{hh}", f"P{pi}"], last=True, skip_group_check=True)
                            if i + 2 < len(pairs):
                                qk(i + 2)
                            if islast:
                                a = acc[hh]
                                ak = f"acc{hh}"
                                if dil == 1:
                                    o = a[:, 512 * c:512 * (c + 1)]
                                    src = ps[ob][:]
                                elif dil == 4:
                                    o = a.rearrange("p (m r) -> p r m", r=4)[:, c, :]
                                    src = ps[ob][:]
                                else:
                                    o = a.rearrange("p (m r) -> p r m", r=16)[:, 4 * c:4 * c + 4, :]
                                    src = ps[ob][:].rearrange("p (r m) -> p r m", r=4)
                                if g == 0:
                                    act(o, src, AF.Copy, [f"ps{ob}"], [ak])
                                else:
                                    tt("dve", o, src, o, ALU.add, [f"ps{ob}", ak], [ak])
                for hh in range(2):
                    fi = rot("F", 7)
                    a = acc[hh]
                    for n in range(4):
                        rec = Fs[fi][0:64, :]
                        p.op("dve", lambda e, rec=rec, a=a, n=n: e.reciprocal(out=rec, in_=a[64:128, n * 512:(n + 1) * 512]), [f"acc{hh}"], [f"F{fi}"])
                        tt("dve", y_attT[hh * 64:(hh + 1) * 64, hp, n * 512:(n + 1) * 512], a[0:64, n * 512:(n + 1) * 512], rec, ALU.mult,
                           [f"acc{hh}", f"F{fi}"], [f"yatt{n}"])

            if stage == "A":
                p.force = True
                for c in range(4):
                    p.dma("pool", "dbg", lambda e, c=c: e.dma_start(out=out_d[c * 256:(c + 1) * 256, :].rearrange("(p two) f -> p (two f)", two=2), in_=y_attT[:, c, :]),
                          [f"yatt{q}" for q in range(4)], [f"dbg{c}"])
                    outkeys.append(f"dbg{c}")
                continue
            for n in range(4):
                gm = rot("Gm", 2)
                mT = G[gm * 2][:, 0:2048]
                mTv = ar2[:, gm * 4096:(gm + 1) * 4096].rearrange("p (c t) -> p c t", c=8)
                mk = [f"G{gm * 2}", f"G{gm * 2 + 1}"]
                for ec in range(8):
                    wga = load_w(BLK_GATE + ec)
                    wgr = load_w(BLK_GATE + 8 + ec)
                    wai = rot("W", 6)
                    load("pool", f"W{wai}", Ws[wai][:, 0:512].rearrange("p (c f) -> p c f", c=4), wa_d[ec], [f"W{wai}"])
                    wbi = rot("W", 6)
                    load("pool", f"W{wbi}", Ws[wbi][:].rearrange("p (c f) -> p c f", c=8), wb_d[ec], [f"W{wbi}"])
                    b1 = rot("psm", 8)
                    proj_fm(wga, n, b1)
                    f1 = rot("F", 7)
                    act(Fs[f1][:], ps[b1][:], AF.Sigmoid, [f"ps{b1}", "sm"], [f"F{f1}"], bias=sm[:, ec:ec + 1])
                    b2 = rot("psm", 8)
                    proj_fm(wgr, n, b2)
                    f2 = rot("F", 7)
                    act(Fs[f2][:], ps[b2][:], AF.Sigmoid, [f"ps{b2}", "sm"], [f"F{f2}"], bias=sm[:, 8 + ec:9 + ec])
                    b3 = rot("psm", 8)
                    wav = Ws[wai][:, 0:512].rearrange("p (c f) -> p c f", c=4)
                    for kc in range(4):
                        mm(b3, ps[b3][:], wav[:, kc, :], y_attT[:, kc, n * 512:(n + 1) * 512], kc == 0, kc == 3, [f"W{wai}", f"yatt{n}"])
                    b4 = rot("psm", 8)
                    wbv = Ws[wbi][:].rearrange("p (c f) -> p c f", c=8)
                    for kc in range(8):
                        mm(b4, ps[b4][:], wbv[:, kc, :], y_retT[:, kc, n * 512:(n + 1) * 512], kc == 0, kc == 7, [f"W{wbi}", f"yret{n}"])
                    tt("dve", Fs[f1][:], ps[b3][:], Fs[f1][:], ALU.mult, [f"ps{b3}", f"F{f1}"], [f"F{f1}"])
                    tt("dve", Fs[f2][:], ps[b4][:], Fs[f2][:], ALU.mult, [f"ps{b4}", f"F{f2}"], [f"F{f2}"])
                    tt("pool", mTv[:, ec, :], Fs[f1][:], Fs[f2][:], ALU.add, [f"F{f1}", f"F{f2}"], mk)
                go = 4 + rot("Go", 2) * 2
                wov = ar2[:, go * 2048:(go + 2) * 2048].rearrange("p (c f) -> p c f", c=8)
                gok = [f"G{go}", f"G{go + 1}"]
                for eh in range(2):
                    load("pool", f"G{go}", wov, wo_d[:, :, eh * 512:(eh + 1) * 512], gok)
                    for t4 in range(4):
                        b = rot("psm", 8)
                        for kc in range(8):
                            mm(b, ps[b][:], mTv[:, kc, t4 * 128:(t4 + 1) * 128], wov[:, kc, :], kc == 0, kc == 7, mk + gok)
                        f = rot("F", 7)
                        r0 = tok0 + n * 512 + t4 * 128
                        load("sp", f"F{f}", Fs[f][:], x_d[r0:r0 + 128, eh * 512:(eh + 1) * 512], [f"F{f}"])
                        tt("dve", Fs[f][:], ps[b][:], Fs[f][:], ALU.add, [f"ps{b}", f"F{f}"], [f"F{f}"])
                        dst = out_d if stage == "h1" else h_d
                        p.dma("sp", f"F{f}", lambda e, f=f, r0=r0, eh=eh, dst=dst: e.dma_start(out=dst[r0:r0 + 128, eh * 512:(eh + 1) * 512], in_=Fs[f][:]),
                              [f"F{f}"], [f"hd{r0}_{eh}"])
                        outkeys.append(f"hd{r0}_{eh}")
            if stage == "h1":
                continue
            p.barrier()
            load("sp", "G7", G[7].bitcast(F32), nw_d[1], ["G7"])
            for t in range(16):
                r0 = tok0 + t * 128
                gi = rot("xg", 2)
                ht = G[gi].bitcast(F32)
                load("sp", f"G{gi}", ht, h_d[r0:r0 + 128, :], [f"G{gi}"], reads=[f"hd{r0}_0", f"hd{r0}_1"])
                fj = rot("F", 7)
                ss = small[:, 8 + (t % 4): 9 + (t % 4)]
                sk = f"ss{t % 4}"
                act(Fs[fj][:].bitcast(BF16), ht, AF.Square, [f"G{gi}"], [f"F{fj}", sk], accum_out=ss)
                rstd_from_ss(ss, 1.0 / D, sk)
                gh = 2 + rot("xb", 2)
                hn = G[gh].bitcast(F32)
                stt(hn, ht, ss, G[7].bitcast(F32), ALU.mult, ALU.mult, [f"G{gi}", sk, "G7"], [f"G{gh}"])
                g32 = 4 + rot("g32", 2)
                h32 = G[g32].bitcast(F32).rearrange("p (c t) -> p c t", c=8)
                for half in range(2):
                    b = rot("psm", 8)
                    for j in range(4):
                        kc = half * 4 + j
                        p.op("pe", lambda e, b=b, j=j, kc=kc, hn=hn: e.transpose(out=ps[b][:, j * 128:(j + 1) * 128], in_=hn[:, kc * 128:(kc + 1) * 128], identity=ident),
                             [f"G{gh}", "c128"], [f"ps{b}"], inc=(j == 3))
                    src = ps[b][:].rearrange("p (c t) -> p c t", c=4)
                    act(xnT[:, half * 4:half * 4 + 4, t * 128:(t + 1) * 128], src, AF.Copy, [f"ps{b}"], [f"xnT{t // 4}"])
                    p.op("dve", lambda e, h32=h32, src=src, half=half: e.tensor_copy(out=h32[:, half * 4:half * 4 + 4, :], in_=src), [f"ps{b}"], [f"G{g32}"])
                b = rot("psm", 8)
                for kc in range(8):
                    mm(b, ps[b][:, 0:72], h32[:, kc, :], wrv[:, kc, :], kc == 0, kc == 7, [f"G{g32}", "wr"])
                fr = rot("F", 7)
                R_ = Fs[fr]
                rk = [f"F{fr}"]
                tt("dve", R_[:, 0:72], ps[b][:, 0:72], br[:], ALU.add, [f"ps{b}", "br"], rk)
                p.op("dve", lambda e, R_=R_: e.tensor_reduce(out=R_[:, 72:73], in_=R_[:, 0:8], axis=AX.X, op=ALU.max), rk, rk)
                ts("dve", R_[:, 73:74], R_[:, 72:73], -1.0, None, ALU.mult, None, rk, rk)
                act(R_[:, 80:88], R_[:, 0:8], AF.Exp, rk, rk, bias=R_[:, 73:74], accum_out=R_[:, 88:89])
                p.op("dve", lambda e, R_=R_: e.reciprocal(out=R_[:, 89:90], in_=R_[:, 88:89]), rk, rk)
                ts("dve", R_[:, 96:104], R_[:, 0:8], R_[:, 72:73], None, ALU.is_equal, None, rk, rk)
                for g_ in range(8):
                    ts("dve", R_[:, 128 + g_ * 8:136 + g_ * 8], R_[:, 8 + g_ * 8:16 + g_ * 8], R_[:, 96 + g_:97 + g_], None, ALU.mult, None, rk, rk,
                       skip_same=(g_ > 0))
                p.op("dve", lambda e, R_=R_: e.tensor_reduce(out=R_[:, 192:200], in_=R_[:, 128:192].rearrange("p (g e) -> p e g", g=8), axis=AX.X, op=ALU.add), rk, rk)
                p.op("dve", lambda e, R_=R_: e.tensor_reduce(out=R_[:, 200:201], in_=R_[:, 192:200], axis=AX.X, op=ALU.max), rk, rk)
                ts("dve", R_[:, 208:216], R_[:, 192:200], R_[:, 200:201], None, ALU.is_equal, None, rk, rk)
                stt(R_[:, 216:224], R_[:, 208:216], -1.0e30, R_[:, 192:200], ALU.mult, ALU.add, rk, rk)
                p.op("dve", lambda e, R_=R_: e.tensor_reduce(out=R_[:, 224:225], in_=R_[:, 216:224], axis=AX.X, op=ALU.max), rk, rk)
                ts("dve", R_[:, 232:240], R_[:, 216:224], R_[:, 224:225], None, ALU.is_equal, None, rk, rk)
                ts("dve", R_[:, 201:202], R_[:, 200:201], -1.0, None, ALU.mult, None, rk, rk)
                act(R_[:, 240:241], R_[:, 224:225], AF.Exp, rk, rk, bias=R_[:, 201:202])
                ts("dve", R_[:, 241:242], R_[:, 240:241], 1.0, None, ALU.add, None, rk, rk)
                p.op("dve", lambda e, R_=R_: e.reciprocal(out=R_[:, 242:243], in_=R_[:, 241:242]), rk, rk)
                tt("dve", R_[:, 243:244], R_[:, 240:241], R_[:, 242:243], ALU.mult, rk, rk)
                ts("dve", R_[:, 244:246], R_[:, 242:244], R_[:, 89:90], None, ALU.mult, None, rk, rk)
                ts("dve", R_[:, 248:256], R_[:, 208:216], R_[:, 244:245], None, ALU.mult, None, rk, rk)
                stt(R_[:, 248:256], R_[:, 232:240], R_[:, 245:246], R_[:, 248:256], ALU.mult, ALU.add, rk, rk)
                for g_ in range(8):
                    ts("dve", R_[:, 256 + g_ * 8:264 + g_ * 8], R_[:, 248:256], R_[:, 96 + g_:97 + g_], None, ALU.mult, None, rk, rk,
                       skip_same=(g_ > 0))
                b = rot("psm", 8)
                p.op("pe", lambda e, b=b, R_=R_: e.transpose(out=ps[b][0:64, 0:128], in_=R_[:, 256:320], identity=ident), rk + ["c128"], [f"ps{b}"])
                act(gateT[:, t * 128:(t + 1) * 128], ps[b][0:64, 0:128], AF.Copy, [f"ps{b}"], ["gateT"])
            p.barrier()
            for q4 in range(4):
                p.op("pool" if q4 % 2 else "dve", lambda e, q4=q4: e.memset(ar1[:, q4 * 8192:(q4 + 1) * 8192].bitcast(F32), 0.0), (), [f"yacc{q4}"])

            def wbuf(bi):
                base = bi * 12288
                return (ar2[:, base:base + 4096].rearrange("p (c f) -> p c f", c=8),
                        ar2[:, base + 4096:base + 8192].rearrange("p (c f) -> p c f", c=8),
                        ar2[:, base + 8192:base + 12288].rearrange("p (c f) -> p c f", c=4))

            def load_expert(e_):
                bi = e_ % 2
                w1v, w3v, w2v = wbuf(bi)
                load("pool", f"EW{bi}a", w1v, w1_d[e_].rearrange("(c p) f -> p c f", p=128), [f"EW{bi}a"])
                load("pool", f"EW{bi}b", w3v, w3_d[e_].rearrange("(c p) f -> p c f", p=128), [f"EW{bi}b"])
                load("pool", f"EW{bi}c", w2v, w2_d[e_].rearrange("(c p) f -> p c f", p=128), [f"EW{bi}c"])

            n_exp = N_EXP
            load_expert(0)
            for e_ in range(n_exp):
                if e_ + 1 < n_exp:
                    load_expert(e_ + 1)
                bi = e_ % 2
                w1v, w3v, w2v = wbuf(bi)
                ts("dve", sel[:], pidx[:], float(e_), None, ALU.is_equal, None, ["pidx"], ["sel"])
                for n in range(4):
                    b = rot("psm", 8)
                    mm(b, ps[b][:], sel[:], gateT[:, n * 512:(n + 1) * 512], True, True, ["sel", "gateT"])
                    fg = rot("F", 7)
                    act(Fs[fg][:], ps[b][:], AF.Copy, [f"ps{b}"], [f"F{fg}"])
                    va_i = rot("actT", 2)
                    aT = vaug[va_i][:].rearrange("p (c t) -> p c t", c=4)
                    for hc in range(4):
                        b1 = rot("psm", 8)
                        for kc in range(8):
                            mm(b1, ps[b1][:], w1v[:, kc, hc * 128:(hc + 1) * 128], xnT[:, kc, n * 512:(n + 1) * 512], kc == 0, kc == 7, [f"EW{bi}a", f"xnT{n}"])
                        b3 = rot("psm", 8)
                        for kc in range(8):
                            mm(b3, ps[b3][:], w3v[:, kc, hc * 128:(hc + 1) * 128], xnT[:, kc, n * 512:(n + 1) * 512], kc == 0, kc == 7, [f"EW{bi}b", f"xnT{n}"])
                        fs_ = rot("F", 7)
                        act(Fs[fs_][:], ps[b1][:], AF.Silu, [f"ps{b1}"], [f"F{fs_}"])
                        tt("dve", Fs[fs_][:], ps[b3][:], Fs[fs_][:], ALU.mult, [f"ps{b3}", f"F{fs_}"], [f"F{fs_}"])
                        tt("pool", aT[:, hc, :], Fs[fs_][:], Fs[fg][:], ALU.mult, [f"F{fs_}", f"F{fg}"], [f"vaug{va_i}"])
                    for tl in range(4):
                        for dh in range(2):
                            b = rot("psm", 8)
                            for hc in range(4):
                                mm(b, ps[b][:], aT[:, hc, tl * 128:(tl + 1) * 128], w2v[:, hc, dh * 512:(dh + 1) * 512], hc == 0, hc == 3,
                                   [f"vaug{va_i}", f"EW{bi}c"])
                            ya = y_acc[:, n * 4 + tl, dh * 512:(dh + 1) * 512]
                            tt("dve", ya, ps[b][:], ya, ALU.add, [f"ps{b}", f"yacc{n}"], [f"yacc{n}"])
            p.barrier()
            load("sp", "G7", G[7].bitcast(F32), nw_d[2], ["G7"])
            for t in range(16):
                r0 = tok0 + t * 128
                gi = rot("xg", 2)
                ht = G[gi].bitcast(F32)
                load("sp", f"G{gi}", ht, h_d[r0:r0 + 128, :], [f"G{gi}"], reads=[f"hd{r0}_0", f"hd{r0}_1"])
                tt("dve", ht, ht, y_acc[:, t, :], ALU.add, [f"G{gi}", f"yacc{t // 4}"], [f"G{gi}"])
                fj = rot("F", 7)
                ss = small[:, 8 + (t % 4): 9 + (t % 4)]
                sk = f"ss{t % 4}"
                act(Fs[fj][:].bitcast(BF16), ht, AF.Square, [f"G{gi}"], [f"F{fj}", sk], accum_out=ss)
                rstd_from_ss(ss, 1.0 / D, sk)
                stt(ht, ht, ss, G[7].bitcast(F32), ALU.mult, ALU.mult, [f"G{gi}", sk, "G7"], [f"G{gi}"])
                p.dma("sp", f"G{gi}", lambda e, r0=r0, ht=ht: e.dma_start(out=out_d[r0:r0 + 128, :], in_=ht), [f"G{gi}"], [f"od{r0}"])
                outkeys.append(f"od{r0}")
            p.barrier()
        p.force = True
        if p.pending["pe"]:
            p.op("pe", lambda e: e.matmul(ps[0][:, 0:128], identb[:], identb[:], start=True, stop=True), [], ["ps0"])
        print("NOPS", p.nops, {e: p.cnt[e] for e in p.ENGS})
        p.wait_all("sp", outkeys)
        p.barrier()
        p.finish()
    return nc


def _consts():
    inv = 1.0 / (10000.0 ** (np.arange(0, 64, 2, dtype=np.float32) / 64.0))
    pos = np.arange(S, dtype=np.float32)
    ang = (pos[None, :] * inv[:, None]).astype(np.float32)
    cosT = np.tile(np.cos(ang), (4, 1)).astype(np.float32)
    sinT = np.tile(np.sin(ang), (4, 1)).astype(np.float32)
    sign = np.where((np.arange(128) % 64) < 32, 1.0, -1.0).astype(np.float32)[:, None]
    cs = np.stack([cosT, sinT * sign]).astype(np.float32)
    j = np.arange(128, dtype=np.float32)[:, None]
    i = np.arange(128, dtype=np.float32)[None, :]
    rel = i - j
    c128 = np.stack([np.eye(128, dtype=np.float32), np.maximum(rel, 0), np.maximum(-rel, 0), rel + 128.0, -rel + 128.0,
                     np.eye(128, dtype=np.float32) * 0.125]).astype(np.float32)
    k = np.arange(128)[:, None]
    q = np.arange(256)[None, :]
    bmask = ((q - k >= 0) & (q - k <= 128)).astype(np.float32)
    iod = np.tile((128.0 * np.arange(16, dtype=np.float32))[None, :], (128, 1)).astype(np.float32)
    pidx = np.tile(np.arange(64, dtype=np.float32)[:, None], (1, 128)).astype(np.float32)
    return cs, c128, bmask, iod, pidx


def _prep(inputs):
    f = lambda a: np.ascontiguousarray(np.asarray(a, dtype=np.float32))
    w_in = f(inputs["w_in"])[0]
    win = np.ascontiguousarray(w_in.reshape(8, 128, 76, 128).transpose(2, 1, 0, 3))
    wa = np.ascontiguousarray(f(inputs["w_attn_branch"])[0].reshape(4, 128, 8, 128).transpose(2, 1, 0, 3))
    wb = np.ascontiguousarray(f(inputs["w_ret_branch"])[0].reshape(8, 128, 8, 128).transpose(2, 1, 0, 3))
    wo = np.ascontiguousarray(f(inputs["w_out"])[0].reshape(8, 128, 1024).transpose(1, 0, 2))
    nw = np.stack([np.tile(f(inputs["norm_mix_w"])[0][None], (128, 1)), np.tile(f(inputs["norm_moe_w"])[0][None], (128, 1)),
                   np.tile(f(inputs["norm_final_w"])[None], (128, 1))]).astype(np.float32)
    sm = np.zeros((128, 64), np.float32)
    sm[:, 0:16] = f(inputs["b_branch_gate"])[0].reshape(16, 128).T
    sm[:, 16:24] = f(inputs["ret_gn_w"])[0].reshape(8, 128).T
    sm[:, 24:32] = f(inputs["ret_decay_fwd"])[0][None, :]
    sm[:, 32:40] = f(inputs["ret_decay_bwd"])[0][None, :]
    wr = np.concatenate([f(inputs["moe_w_group"])[0], f(inputs["moe_w_expert"])[0]], axis=1)
    wr = np.ascontiguousarray(wr.reshape(8, 128, 72).transpose(1, 0, 2))
    br = np.tile(np.concatenate([f(inputs["moe_b_group"])[0], f(inputs["moe_b_expert"])[0]])[None], (128, 1)).astype(np.float32)
    cs, c128, bmask, iod, pidx = _consts()
    shared = dict(win=win, wa=wa, wb=wb, wo=wo, nw=nw, cs=cs, sm=sm, c128=c128, bm=bmask, iod=iod, wr=wr, br=br, pidx=pidx,
                  w1=f(inputs["moe_w1"])[0], w3=f(inputs["moe_w3"])[0], w2=f(inputs["moe_w2"])[0])
    x = f(inputs["x"])
    maps = []
    for c in range(8):
        m = dict(shared)
        m["x"] = np.ascontiguousarray(x[2 * c:2 * c + 2].reshape(NSEQ * S, D))
        maps.append(m)
    return maps


_NC_CACHE = {}


def kernel(**inputs):
    maps = _prep(inputs)
    if "full" not in _NC_CACHE:
        _NC_CACHE["full"] = build("full")
    nc = _NC_CACHE["full"]
    res = run_bass_kernel_spmd(nc, maps, core_ids=list(range(8)))
    out = np.stack([r["out"].reshape(NSEQ, S, D) for r in res.results]).reshape(16, S, D)
    return out.astype(np.float32)
```
